# Optimizing a Trainium2 kernel written in Bass

```python
import jax, jax.numpy as jnp
from jax import lax
import numpy as np

D_MODEL = 1024
BATCH = 8
SEQ = 2048
DEPTH = 4

N_MIXERS = 3
DEEPNORM_ALPHA = (2.0 * DEPTH) ** 0.25
DEEPNORM_BETA = (8.0 * DEPTH) ** -0.25
LN_EPS = 1e-5
NEG_INF = -1e30

CONV_KERNEL = 31
SHORT_KERNEL = 3
NSA_HEADS = 16
NSA_KV_HEADS = 4
NSA_GROUP = NSA_HEADS // NSA_KV_HEADS
HEAD_DIM = D_MODEL // NSA_HEADS
CMP_BLOCK = 32
CMP_STRIDE = 16
SLC_BLOCK = 64
SLC_TOP_N = 16
SLC_FORCED_LOCAL = 2
FORCE_BONUS = 1e4
WINDOW = 512
PHI_HIDDEN = 2 * HEAD_DIM
NSA_QBLK = 64
NSA_IN_COLS = NSA_HEADS * HEAD_DIM + 6 * NSA_KV_HEADS * HEAD_DIM + 3 * NSA_HEADS
D_FF = 2816
N_EXPERTS = 8
TOP_K = 2
D_FF_EXPERT = 3584

kernel_name = "hybrid_conformer_shortconv_nsa_moe_deepnorm"


def _count(period, phase):
    return len(range(phase, DEPTH, period))


def _layer_norm(x, g, b):
    xf = x.astype(jnp.float32)
    mu = jnp.mean(xf, -1, keepdims=True)
    var = jnp.mean(jnp.square(xf - mu), -1, keepdims=True)
    y = (xf - mu) * lax.rsqrt(var + LN_EPS)
    return (y * g.astype(jnp.float32) + b.astype(jnp.float32)).astype(x.dtype)


def _causal_depthwise_conv(x, w):
    k = w.shape[0]
    return lax.conv_general_dilated(
        x, w[:, None, :], window_strides=(1,), padding=[(k - 1, 0)],
        dimension_numbers=("NWC", "WIO", "NWC"), feature_group_count=x.shape[-1])


def _masked_softmax(s, mask):
    s = jnp.where(mask, s.astype(jnp.float32), NEG_INF)
    m = jnp.max(s, -1, keepdims=True)
    e = jnp.exp(s - m) * mask
    return e / jnp.maximum(jnp.sum(e, -1, keepdims=True), 1e-30)


def conformer_conv(x, w_in, w_dw, b_dw, ln_g, ln_b, w_out):
    a, gate = jnp.split(x @ w_in, 2, axis=-1)
    u = a * jax.nn.sigmoid(gate)
    u = _causal_depthwise_conv(u, w_dw) + b_dw
    u = jax.nn.silu(_layer_norm(u, ln_g, ln_b))
    return u @ w_out


def short_gated_conv(x, w_in, w_conv, w_out):
    b_gate, c_gate, h = jnp.split(x @ w_in, 3, axis=-1)
    y = b_gate * _causal_depthwise_conv(c_gate * h, w_conv)
    return y @ w_out


def _compress(kv, pe, w1, w2):
    B, T, Hk, dh = kv.shape
    n_cmp = (T - CMP_BLOCK) // CMP_STRIDE + 1
    idx = jnp.arange(n_cmp)[:, None] * CMP_STRIDE + jnp.arange(CMP_BLOCK)[None, :]
    blocks = kv[:, idx] + pe[:, None, :]
    blocks = jnp.moveaxis(blocks, 3, 1).reshape(B, Hk, n_cmp, CMP_BLOCK * dh)
    return jax.nn.gelu(blocks @ w1) @ w2


def native_sparse_attention(x, w_in, pe_k, pe_v, wk1, wk2, wv1, wv2, w_out):
    B, T, _ = x.shape
    H, Hk, G, dh = NSA_HEADS, NSA_KV_HEADS, NSA_GROUP, HEAD_DIM
    qd, kd = H * dh, Hk * dh
    cuts = [qd, qd + kd, qd + 2 * kd, qd + 3 * kd, qd + 4 * kd, qd + 5 * kd, qd + 6 * kd]
    q, kc, vc, ks, vs, kw, vw, g = jnp.split(x @ w_in, cuts, axis=-1)
    q = q.reshape(B, T, Hk, G, dh).transpose(0, 2, 3, 1, 4) * (dh ** -0.5)
    kv_shape = (B, T, Hk, dh)
    kc, vc = kc.reshape(kv_shape), vc.reshape(kv_shape)
    ks = ks.reshape(kv_shape).transpose(0, 2, 1, 3)
    vs = vs.reshape(kv_shape).transpose(0, 2, 1, 3)
    kw = kw.reshape(kv_shape).transpose(0, 2, 1, 3)
    vw = vw.reshape(kv_shape).transpose(0, 2, 1, 3)
    t_pos = jnp.arange(T)

    k_cmp = _compress(kc, pe_k, wk1, wk2)
    v_cmp = _compress(vc, pe_v, wv1, wv2)
    n_cmp = k_cmp.shape[2]
    cmp_start = jnp.arange(n_cmp) * CMP_STRIDE
    mask_c = (cmp_start + CMP_BLOCK - 1)[None, :] <= t_pos[:, None]
    p_cmp = _masked_softmax(jnp.einsum("bhgtd,bhcd->bhgtc", q, k_cmp), mask_c)
    o_cmp = jnp.einsum("bhgtc,bhcd->bhgtd", p_cmp.astype(v_cmp.dtype), v_cmp)

    n_slc = T // SLC_BLOCK
    blk = jnp.arange(n_slc)
    slc_start = blk * SLC_BLOCK
    overlap = ((cmp_start[:, None] < (slc_start + SLC_BLOCK)[None, :]) &
               ((cmp_start + CMP_BLOCK)[:, None] > slc_start[None, :])).astype(jnp.float32)
    imp = jnp.einsum("bhgtc,cn->bhtn", p_cmp, overlap)
    valid = slc_start[None, :] <= t_pos[:, None]
    d_cur = (t_pos // SLC_BLOCK)[:, None] - blk[None, :]
    forced = (blk[None, :] == 0) | ((d_cur >= 0) & (d_cur < SLC_FORCED_LOCAL))
    score = jnp.where(forced, FORCE_BONUS, jnp.where(valid, imp, NEG_INF))
    top_n = min(SLC_TOP_N, n_slc)
    _, sel = lax.top_k(score, top_n)

    ks_blk = ks.reshape(B, Hk, n_slc, SLC_BLOCK, dh)
    vs_blk = vs.reshape(B, Hk, n_slc, SLC_BLOCK, dh)
    kw_pad = jnp.pad(kw, ((0, 0), (0, 0), (WINDOW, 0), (0, 0)))
    vw_pad = jnp.pad(vw, ((0, 0), (0, 0), (WINDOW, 0), (0, 0)))
    QB = NSA_QBLK
    nq = T // QB
    q_blocks = jnp.moveaxis(q.reshape(B, Hk, G, nq, QB, dh), 3, 0)
    sel_blocks = jnp.moveaxis(sel.reshape(B, Hk, nq, QB, top_n), 2, 0)
    bi = jnp.arange(B)[:, None, None, None]
    hi = jnp.arange(Hk)[None, :, None, None]

    def local_branches(args):
        qi, q_b, sel_b = args
        t_b = qi * QB + jnp.arange(QB)
        k_sel = ks_blk[bi, hi, sel_b]
        v_sel = vs_blk[bi, hi, sel_b].reshape(B, Hk, QB, top_n * SLC_BLOCK, dh)
        pos = sel_b[..., None] * SLC_BLOCK + jnp.arange(SLC_BLOCK)
        mask_s = (pos <= t_b[:, None, None]).reshape(B, Hk, 1, QB, top_n * SLC_BLOCK)
        s_sel = jnp.einsum("bhgqd,bhqnsd->bhgqns", q_b, k_sel).reshape(B, Hk, G, QB, top_n * SLC_BLOCK)
        p_sel = _masked_softmax(s_sel, mask_s)
        o_sel = jnp.einsum("bhgqk,bhqkd->bhgqd", p_sel.astype(v_sel.dtype), v_sel)
        k_win = lax.dynamic_slice_in_dim(kw_pad, qi * QB, WINDOW + QB, axis=2)
        v_win = lax.dynamic_slice_in_dim(vw_pad, qi * QB, WINDOW + QB, axis=2)
        kpos = qi * QB - WINDOW + jnp.arange(WINDOW + QB)
        diff = t_b[:, None] - kpos[None, :]
        mask_w = (diff >= 0) & (diff < WINDOW) & (kpos[None, :] >= 0)
        p_win = _masked_softmax(jnp.einsum("bhgqd,bhkd->bhgqk", q_b, k_win), mask_w)
        o_win = jnp.einsum("bhgqk,bhkd->bhgqd", p_win.astype(v_win.dtype), v_win)
        return o_sel, o_win

    o_sel, o_win = lax.map(local_branches, (jnp.arange(nq), q_blocks, sel_blocks))
    o_sel = jnp.moveaxis(o_sel, 0, 3).reshape(B, Hk, G, T, dh)
    o_win = jnp.moveaxis(o_win, 0, 3).reshape(B, Hk, G, T, dh)

    gates = jax.nn.sigmoid(g.reshape(B, T, Hk, G, 3).transpose(0, 2, 3, 1, 4))
    o = gates[..., 0:1] * o_cmp + gates[..., 1:2] * o_sel + gates[..., 2:3] * o_win
    o = o.transpose(0, 3, 1, 2, 4).reshape(B, T, H * dh)
    return o @ w_out


def swiglu(x, w_gate, w_up, w_down):
    return (jax.nn.silu(x @ w_gate) * (x @ w_up)) @ w_down


def moe_swiglu(x, w_router, w_gate, w_up, w_down):
    logits = (x @ w_router).astype(jnp.float32)
    top_val, top_idx = lax.top_k(logits, TOP_K)
    wts = jax.nn.softmax(top_val, axis=-1)
    gates = jnp.sum(jax.nn.one_hot(top_idx, N_EXPERTS, dtype=jnp.float32) * wts[..., None], axis=-2)
    out = jnp.zeros_like(x)
    for e in range(N_EXPERTS):
        h = jax.nn.silu(x @ w_gate[e]) * (x @ w_up[e])
        out = out + gates[..., e:e + 1].astype(x.dtype) * (h @ w_down[e])
    return out


def setup_inputs(seed: int = 0) -> dict:
    key = jax.random.key(seed)
    ks = iter(jax.random.split(key, 40))
    f32 = jnp.float32
    D, dh = D_MODEL, HEAD_DIM
    nA, nB, nC = _count(N_MIXERS, 0), _count(N_MIXERS, 1), _count(N_MIXERS, 2)
    nDense, nMoE = _count(2, 0), _count(2, 1)

    def nrm(shape, scale):
        return jax.random.normal(next(ks), shape, f32) * scale

    def gain(shape):
        return 1.0 + nrm(shape, 0.02)

    beta = DEEPNORM_BETA
    return {
        "x": nrm((BATCH, SEQ, D), 1.0),
        "ln_mix_g": gain((DEPTH, D)),
        "ln_mix_b": nrm((DEPTH, D), 0.02),
        "ln_ffn_g": gain((DEPTH, D)),
        "ln_ffn_b": nrm((DEPTH, D), 0.02),
        "conf_w_in": nrm((nA, D, 2 * D), D ** -0.5),
        "conf_w_dw": nrm((nA, CONV_KERNEL, D), CONV_KERNEL ** -0.5),
        "conf_b_dw": nrm((nA, D), 0.02),
        "conf_ln_g": gain((nA, D)),
        "conf_ln_b": nrm((nA, D), 0.02),
        "conf_w_out": nrm((nA, D, D), beta * D ** -0.5),
        "sc_w_in": nrm((nB, D, 3 * D), D ** -0.5),
        "sc_w_conv": nrm((nB, SHORT_KERNEL, D), SHORT_KERNEL ** -0.5),
        "sc_w_out": nrm((nB, D, D), beta * D ** -0.5),
        "nsa_w_in": nrm((nC, D, NSA_IN_COLS), D ** -0.5),
        "nsa_pe_k": nrm((nC, CMP_BLOCK, dh), 0.1),
        "nsa_pe_v": nrm((nC, CMP_BLOCK, dh), 0.1),
        "nsa_wk1": nrm((nC, CMP_BLOCK * dh, PHI_HIDDEN), (CMP_BLOCK * dh) ** -0.5),
        "nsa_wk2": nrm((nC, PHI_HIDDEN, dh), PHI_HIDDEN ** -0.5),
        "nsa_wv1": nrm((nC, CMP_BLOCK * dh, PHI_HIDDEN), (CMP_BLOCK * dh) ** -0.5),
        "nsa_wv2": nrm((nC, PHI_HIDDEN, dh), PHI_HIDDEN ** -0.5),
        "nsa_w_out": nrm((nC, D, D), beta * D ** -0.5),
        "ffn_w_gate": nrm((nDense, D, D_FF), D ** -0.5),
        "ffn_w_up": nrm((nDense, D, D_FF), D ** -0.5),
        "ffn_w_down": nrm((nDense, D_FF, D), beta * D_FF ** -0.5),
        "moe_w_router": nrm((nMoE, D, N_EXPERTS), D ** -0.5),
        "moe_w_gate": nrm((nMoE, N_EXPERTS, D, D_FF_EXPERT), D ** -0.5),
        "moe_w_up": nrm((nMoE, N_EXPERTS, D, D_FF_EXPERT), D ** -0.5),
        "moe_w_down": nrm((nMoE, N_EXPERTS, D_FF_EXPERT, D), beta * D_FF_EXPERT ** -0.5),
    }


def reference(x, ln_mix_g, ln_mix_b, ln_ffn_g, ln_ffn_b,
              conf_w_in, conf_w_dw, conf_b_dw, conf_ln_g, conf_ln_b, conf_w_out,
              sc_w_in, sc_w_conv, sc_w_out,
              nsa_w_in, nsa_pe_k, nsa_pe_v, nsa_wk1, nsa_wk2, nsa_wv1, nsa_wv2, nsa_w_out,
              ffn_w_gate, ffn_w_up, ffn_w_down,
              moe_w_router, moe_w_gate, moe_w_up, moe_w_down):
    for i in range(DEPTH):
        kind, j = i % N_MIXERS, i // N_MIXERS
        if kind == 0:
            mix = conformer_conv(x, conf_w_in[j], conf_w_dw[j], conf_b_dw[j],
                                 conf_ln_g[j], conf_ln_b[j], conf_w_out[j])
        elif kind == 1:
            mix = short_gated_conv(x, sc_w_in[j], sc_w_conv[j], sc_w_out[j])
        else:
            mix = native_sparse_attention(x, nsa_w_in[j], nsa_pe_k[j], nsa_pe_v[j],
                                          nsa_wk1[j], nsa_wk2[j], nsa_wv1[j], nsa_wv2[j],
                                          nsa_w_out[j])
        x = _layer_norm(DEEPNORM_ALPHA * x + mix, ln_mix_g[i], ln_mix_b[i])
        f = i // 2
        if i % 2 == 0:
            ffn = swiglu(x, ffn_w_gate[f], ffn_w_up[f], ffn_w_down[f])
        else:
            ffn = moe_swiglu(x, moe_w_router[f], moe_w_gate[f], moe_w_up[f], moe_w_down[f])
        x = _layer_norm(DEEPNORM_ALPHA * x + ffn, ln_ffn_g[i], ln_ffn_b[i])
    return x
```

```python
import numpy as np
import ml_dtypes
from contextlib import ExitStack
import concourse.bass as bass
import concourse.mybir as mybir
from concourse.bass_utils import run_bass_kernel_spmd

F32 = mybir.dt.float32
BF16 = mybir.dt.bfloat16
AF = mybir.ActivationFunctionType
ALU = mybir.AluOpType

T = 2048
D = 1024
NT = 16
NK = 8
DEPTH = 4
ALPHA = float((2.0 * DEPTH) ** 0.25)
EPS = 1e-5
D_FF = 2816
D_FFE = 3584
NE = 8
CONVK = 31
ARENA_F32 = 26176


class Sched:
    def __init__(self, nc, es):
        self.nc = nc
        self.eng = dict(pe=nc.tensor, act=nc.scalar, dve=nc.vector, pool=nc.gpsimd, sp=nc.sync)
        self.semobj = {}
        for k in self.eng:
            self.semobj[k] = es.enter_context(nc.semaphore("c_" + k))
        self.cnt = {k: 0 for k in self.eng}
        self.seen = {k: {} for k in self.eng}
        self.dq = {}
        for q, n in (("sp", 8), ("pool", 4), ("act", 2)):
            lst = []
            for i in range(n):
                key = "d_%s%d" % (q, i)
                self.semobj[key] = es.enter_context(nc.semaphore(key))
                lst.append([key, 0])
            self.dq[q] = lst
        self.dq_next = {q: 0 for q in self.dq}
        self.lastw = {}
        self.readers = {}
        self.nwaits = 0
        self.nops = 0

    def wait(self, E, tok):
        key, val = tok
        if self.seen[E].get(key, 0) >= val:
            return
        self.eng[E].wait_ge(self.semobj[key], val)
        self.seen[E][key] = val
        self.nwaits += 1

    def _deps(self, E, reads, writes, is_dma):
        deps = {}

        def add(tok, raw):
            key, val = tok
            if not is_dma and key == E:
                if E == "pe" or not raw:
                    return
            if deps.get(key, 0) < val:
                deps[key] = val

        for p in reads:
            t = self.lastw.get(p)
            if t is not None:
                add(t, True)
        for p in writes:
            t = self.lastw.get(p)
            if t is not None:
                add(t, False)
            r = self.readers.get(p)
            if r:
                for k, v in r.items():
                    add((k, v), False)
        for k, v in deps.items():
            self.wait(E, (k, v))

    def _commit(self, tok, reads, writes):
        key, val = tok
        for p in reads:
            r = self.readers.setdefault(p, {})
            if r.get(key, 0) < val:
                r[key] = val
        for p in writes:
            self.lastw[p] = tok
            self.readers[p] = {}

    def op(self, E, reads, writes, fn):
        self._deps(E, reads, writes, False)
        ins = fn(self.eng[E])
        self.cnt[E] += 1
        ins.then_inc(self.semobj[E], 1)
        self._commit((E, self.cnt[E]), reads, writes)
        self.nops += 1

    def dma(self, Q, reads, writes, out, in_, **kw):
        slot = self.dq[Q][self.dq_next[Q]]
        self.dq_next[Q] = (self.dq_next[Q] + 1) % len(self.dq[Q])
        key, cur = slot
        if cur > 0:
            self.wait(Q, (key, cur))
        self._deps(Q, reads, writes, True)
        ins = self.eng[Q].dma_start(out=out, in_=in_, **kw)
        ins.then_inc(self.semobj[key], 16)
        slot[1] = cur + 16
        tok = (key, cur + 16)
        self._commit(tok, reads, writes)
        self.nops += 1
        return tok

    def barrier(self, engines=("pe", "act", "dve", "pool", "sp")):
        for E in engines:
            for F in ("pe", "act", "dve", "pool"):
                if F != E and self.cnt[F] > 0:
                    self.wait(E, (F, self.cnt[F]))
            for q in self.dq:
                for key, cur in self.dq[q]:
                    if cur > 0:
                        self.wait(E, (key, cur))


def P(name, *idx):
    return (name,) + idx


class Ctx:
    pass


def build_program(n_layers=DEPTH, layers=None):
    nc = bass.Bass("TRN2", target_bir_lowering=False)
    es = ExitStack()
    C = Ctx()
    C.nc = nc

    def din(name, shape, dt=F32):
        return nc.dram_tensor(name, list(shape), dt, kind="ExternalInput").ap()

    dr = {}
    dr["x"] = din("x", [T, D])
    for n in ("ln_mix_g", "ln_mix_b", "ln_ffn_g", "ln_ffn_b"):
        dr[n] = din(n, [DEPTH, D])
    dr["conf_w_in"] = din("conf_w_in", [2, D, 2 * D])
    dr["conf_w_dwT"] = din("conf_w_dwT", [2, D, CONVK])
    dr["conf_vec"] = din("conf_vec", [2, 128, 3, NK])
    dr["conf_w_out"] = din("conf_w_out", [2, D, D])
    dr["sc_w_in"] = din("sc_w_in", [1, D, 3 * D])
    dr["sc_w_convT"] = din("sc_w_convT", [1, D, 3])
    dr["sc_w_out"] = din("sc_w_out", [1, D, D])
    dr["ffn_w_gate"] = din("ffn_w_gate", [2, D, D_FF])
    dr["ffn_w_up"] = din("ffn_w_up", [2, D, D_FF])
    dr["ffn_w_down"] = din("ffn_w_down", [2, D_FF, D])
    dr["moe_w_router"] = din("moe_w_router", [2, D, NE])
    dr["moe_w_gate"] = din("moe_w_gate", [2, NE, D, D_FFE])
    dr["moe_w_up"] = din("moe_w_up", [2, NE, D, D_FFE])
    dr["moe_w_down"] = din("moe_w_down", [2, NE, D_FFE, D])
    dr["nsa_w_in"] = din("nsa_w_in", [1, D, 2608])
    dr["nsa_pe_kT"] = din("nsa_pe_kT", [1, 64, 32])
    dr["nsa_pe_vT"] = din("nsa_pe_vT", [1, 64, 32])
    dr["nsa_wk1"] = din("nsa_wk1", [1, 2048, 128])
    dr["nsa_wv1"] = din("nsa_wv1", [1, 2048, 128])
    dr["nsa_wk2"] = din("nsa_wk2", [1, 128, 64])
    dr["nsa_wv2"] = din("nsa_wv2", [1, 128, 64])
    dr["nsa_w_out"] = din("nsa_w_out", [1, D, D])
    dr["c_cmpmask"] = din("c_cmpmask", [128, 2048], BF16)
    dr["c_caus"] = din("c_caus", [128, 4, 512], BF16)
    dr["c_winm"] = din("c_winm", [128, 4, 512], BF16)
    dr["c_exp2"] = din("c_exp2", [32, 16, 128], BF16)
    dr["c_vnf"] = din("c_vnf", [128, 16, 32], BF16)
    dr["c_bonus"] = din("c_bonus", [128, 16, 32], BF16)
    dr["c_onesovl"] = din("c_onesovl", [128, 33], BF16)
    dr["ident_bf"] = din("ident_bf", [128, 128], BF16)
    dr["ones_bf"] = din("ones_bf", [128, 128], BF16)
    out = nc.dram_tensor("out", [T, D], F32, kind="ExternalOutput").ap()
    C.dr = dr

    X = es.enter_context(nc.sbuf_tensor("X", [128, NT, D], F32))
    XT = es.enter_context(nc.sbuf_tensor("XT", [128, NK, T], BF16))
    ARENA = es.enter_context(nc.sbuf_tensor("ARENA", [128, ARENA_F32], F32))
    LNG = es.enter_context(nc.sbuf_tensor("LNG", [128, D], F32))
    LNB = es.enter_context(nc.sbuf_tensor("LNB", [128, D], F32))
    IDB = es.enter_context(nc.sbuf_tensor("IDB", [128, 128], BF16))
    ONES = es.enter_context(nc.sbuf_tensor("ONES", [128, 128], BF16))
    SMALL = es.enter_context(nc.sbuf_tensor("SMALL", [128, 64], F32))
    EPST = es.enter_context(nc.sbuf_tensor("EPST", [128, 1], F32))
    PS = es.enter_context(nc.psum_tensor("PS", [128, 4096], F32))
    S = Sched(nc, es)
    C.S = S
    C.X, C.XT, C.ARENA, C.PS = X, XT, ARENA, PS
    C.LNG, C.LNB, C.IDB, C.ONES, C.SMALL, C.EPST = LNG, LNB, IDB, ONES, SMALL, EPST

    def bank(b):
        return PS[:, b * 512:(b + 1) * 512]

    C.bank = bank

    def aview(off, n32, dt=F32, **re):
        v = ARENA[:, off:off + n32]
        if dt != F32:
            v = v.bitcast(dt)
        if re:
            pat = re.pop("pat")
            v = v.rearrange(pat, **re)
        return v

    C.aview = aview

    S.dma("sp", [], [P("IDB")], IDB[:], dr["ident_bf"][:, :])
    S.dma("sp", [], [P("ONES")], ONES[:], dr["ones_bf"][:, :])
    S.op("dve", [], [P("EPST")], lambda e: e.memset(EPST[:], EPS))
    xv = dr["x"].rearrange("(t p) d -> p t d", p=128)
    for tt in range(NT):
        S.dma("sp", [], [P("X", tt)], X[:, tt, :], xv[:, tt, :])
    for tt in range(NT):
        make_xt(C, tt)

    for li in (range(n_layers) if layers is None else layers):
        kind, j = li % 3, li // 3
        if (ONLY_NSA and li != 2) or li in SKIP_LAYERS:
            continue
        if kind == 0:
            conformer(C, li, j)
        elif kind == 1:
            shortconv(C, li, 0)
        else:
            nsa(C, li, 0)
        f = li // 2
        if li % 2 == 0:
            ffn(C, li, [(dr["ffn_w_gate"][f], dr["ffn_w_up"][f], dr["ffn_w_down"][f], D_FF)], None)
        else:
            moe(C, li, f)

    S.barrier(("sp",))
    ov = out.rearrange("(t p) d -> p t d", p=128)
    toks = []
    for tt in range(NT):
        toks.append(S.dma("sp", [P("X", tt)], [], ov[:, tt, :], X[:, tt, :]))
    for q in S.dq:
        for key, cur in S.dq[q]:
            if cur > 0:
                S.wait("sp", (key, cur))
    C.es = es
    return nc, C


def make_xt(C, tt, tmp_off=None):
    S, X, XT = C.S, C.X, C.XT
    slot = tt % 2
    off = ARENA_F32 - 1024 + slot * 512
    xbv = C.ARENA[:, off:off + 512].bitcast(BF16)
    pxb = P("xb", slot)
    S.op("act", [P("X", tt)], [pxb], lambda e: e.activation(out=xbv, in_=X[:, tt, :], func=AF.Copy))
    b = slot
    pst = C.bank(b).bitcast(BF16)
    for c in range(NK):
        S.op("pe", [pxb, P("IDB")], [P("ps", b)],
             lambda e, c=c: e.transpose(out=pst[:, c * 128:(c + 1) * 128], in_=xbv[:, c * 128:(c + 1) * 128],
                                        identity=C.IDB[:]))
    S.op("dve", [P("ps", b)], [P("XT", tt)],
         lambda e: e.tensor_copy(out=XT[:, :, tt * 128:(tt + 1) * 128],
                                 in_=pst.rearrange("p (c n) -> p c n", n=128)))


def load_ln(C, g_ap, b_ap):
    S = C.S
    S.dma("sp", [], [P("LNG")], C.LNG[:], g_ap.partition_broadcast(128))
    S.dma("sp", [], [P("LNB")], C.LNB[:], b_ap.partition_broadcast(128))


def ln_tail(C, tt):
    S, X = C.S, C.X
    sm = C.SMALL
    slot = tt % 2
    so = slot * 32
    stats = sm[:, so:so + 12]
    mv = sm[:, so + 12:so + 14]
    std = sm[:, so + 14:so + 15]
    rstd = sm[:, so + 15:so + 16]
    nmr = sm[:, so + 16:so + 17]
    pst = P("lnsm", slot)
    xt_ = X[:, tt, :]
    S.op("dve", [P("X", tt)], [pst], lambda e: e.bn_stats(out=stats[:, 0:6], in_=xt_[:, 0:512]))
    S.op("dve", [P("X", tt)], [pst], lambda e: e.bn_stats(out=stats[:, 6:12], in_=xt_[:, 512:1024]))
    S.op("dve", [pst], [pst], lambda e: e.bn_aggr(out=mv, in_=stats))
    S.op("act", [pst, P("EPST")], [pst],
         lambda e: e.activation(out=std, in_=mv[:, 1:2], func=AF.Sqrt, bias=C.EPST[:], scale=1.0))
    S.op("dve", [pst], [pst], lambda e: e.reciprocal(out=rstd, in_=std))
    S.op("dve", [pst], [pst],
         lambda e: e.scalar_tensor_tensor(out=nmr, in0=mv[:, 0:1], scalar=-1.0, in1=rstd,
                                          op0=ALU.mult, op1=ALU.mult))
    S.op("act", [pst, P("X", tt)], [P("X", tt)],
         lambda e: e.activation(out=xt_, in_=xt_, func=AF.Identity, bias=nmr, scale=rstd))
    S.op("dve", [P("X", tt), P("LNG")], [P("X", tt)],
         lambda e: e.tensor_tensor(out=xt_, in0=xt_, in1=C.LNG[:], op=ALU.mult))
    S.op("dve", [P("X", tt), P("LNB")], [P("X", tt)],
         lambda e: e.tensor_tensor(out=xt_, in0=xt_, in1=C.LNB[:], op=ALU.add))
    make_xt(C, tt)


def out_proj_ln(C, li, actT, pact, wout_dram, off_w, extra_w=()):
    S, X = C.S, C.X
    wo = C.aview(off_w, 4096, BF16, pat="p (k n) -> p k n", n=D)
    wv = wout_dram.rearrange("(k p) n -> p k n", p=128)
    for h in range(2):
        S.dma("pool", [], [P("wo", h)] + list(extra_w), wo[:, :, h * 512:(h + 1) * 512], wv[:, :, h * 512:(h + 1) * 512])
    load_ln(C, C.dr["ln_mix_g"][li], C.dr["ln_mix_b"][li])
    for tt in range(NT):
        bs = 4 + 2 * (tt % 2)
        for h in range(2):
            for k in range(NK):
                S.op("pe", [pact(k), P("wo", h)], [P("ps", bs + h)],
                     lambda e, k=k, h=h: e.matmul(C.bank(bs + h), lhsT=actT[:, k, tt * 128:(tt + 1) * 128],
                                                  rhs=wo[:, k, h * 512:(h + 1) * 512],
                                                  start=(k == 0), stop=(k == NK - 1)))
        S.op("dve", [P("ps", bs), P("ps", bs + 1), P("X", tt)], [P("X", tt)],
             lambda e: e.scalar_tensor_tensor(out=X[:, tt, :], in0=X[:, tt, :], scalar=ALPHA,
                                              in1=C.PS[:, bs * 512:bs * 512 + 1024],
                                              op0=ALU.mult, op1=ALU.add))
        ln_tail(C, tt)


def conformer(C, li, j):
    S, nc, dr = C.S, C.nc, C.dr
    S.barrier()
    XT = C.XT
    o = 0
    V = C.aview(o, 8192, BF16, pat="p (c t) -> p c t", t=T); o += 8192
    WIN = [C.aview(o + i * 2048, 2048, BF16, pat="p (k n) -> p k n", n=512) for i in range(3)]; o_w = o; o += 6144
    U = [C.aview(o + i * 2080, 2080) for i in range(2)]; o += 4160
    ACC = C.aview(o, 2048); o += 2048
    MR = C.aview(o, 1024); o += 1024
    SQ = [C.aview(o + i * 256, 256, BF16) for i in range(2)]; o += 512
    ZT = [C.aview(o + i * 512, 512) for i in range(2)]; o += 1024
    SG = [C.aview(o + i * 512, 512) for i in range(2)]; o += 1024
    WDW = C.aview(o, 256, pat="p (c k) -> p c k", k=32); o += 256
    VEC = C.aview(o, 24, pat="p (w c) -> p w c", c=NK); o += 24
    assert o <= ARENA_F32 - 1024
    for c in range(NK):
        S.dma("sp", [], [P("wdw")], WDW[:, c, 0:CONVK], dr["conf_w_dwT"][j, c * 128:(c + 1) * 128, :])
    S.dma("sp", [], [P("vec")], VEC[:], dr["conf_vec"][j])
    for i in range(2):
        S.op("dve", [], [P("U", i)], lambda e, i=i: e.memset(U[i][:, 0:32], 0.0))
    win_d = dr["conf_w_in"][j].rearrange("(k p) n -> p k n", p=128)
    for grp in range(4):
        wt = WIN[grp % 3]
        pw = P("win", grp % 3)
        S.dma("pool", [], [pw], wt[:, :, 0:256], win_d[:, :, grp * 256:(grp + 1) * 256])
        S.dma("pool", [], [pw], wt[:, :, 256:512], win_d[:, :, D + grp * 256:D + (grp + 1) * 256])
        for cc in range(2):
            oc = grp * 2 + cc
            ub = U[oc % 2]
            pu = P("U", oc % 2)
            for tb in range(4):
                bset = 2 * ((oc * 4 + tb) % 2)
                for which in range(2):
                    for k in range(NK):
                        S.op("pe", [pw] + [P("XT", t4) for t4 in range(tb * 4, tb * 4 + 4)], [P("ps", bset + which)],
                             lambda e, k=k, which=which: e.matmul(
                                 C.bank(bset + which), lhsT=wt[:, k, which * 256 + cc * 128: which * 256 + cc * 128 + 128],
                                 rhs=XT[:, k, tb * 512:(tb + 1) * 512], start=(k == 0), stop=(k == NK - 1)))
                sg = SG[tb % 2]
                S.op("act", [P("ps", bset + 1)], [P("sg", tb % 2)],
                     lambda e: e.activation(out=sg, in_=C.bank(bset + 1), func=AF.Sigmoid))
                S.op("dve", [P("ps", bset), P("sg", tb % 2)], [pu],
                     lambda e: e.tensor_tensor(out=ub[:, 32 + tb * 512: 32 + (tb + 1) * 512], in0=C.bank(bset),
                                               in1=sg, op=ALU.mult))
            S.op("dve", [pu, P("wdw"), P("vec")], [P("acc")],
                 lambda e: e.tensor_scalar(out=ACC, in0=ub[:, 2:2 + T], scalar1=WDW[:, oc, 0:1],
                                           scalar2=VEC[:, 0, oc:oc + 1], op0=ALU.mult, op1=ALU.add))
            for k in range(1, CONVK):
                last = (k == CONVK - 1)
                dst = V[:, oc, :] if last else ACC
                pd = P("V", oc) if last else P("acc")
                S.op("dve", [pu, P("wdw"), P("acc")], [pd],
                     lambda e, k=k, dst=dst: e.scalar_tensor_tensor(out=dst, in0=ub[:, 2 + k:2 + k + T],
                                                                    scalar=WDW[:, oc, k:k + 1], in1=ACC,
                                                                    op0=ALU.mult, op1=ALU.add))
    for tb in range(4):
        sl = slice(tb * 512, (tb + 1) * 512)
        b1, b2 = 0, 1
        for c in range(NK):
            sq = SQ[c % 2]
            S.op("dve", [P("V", c)], [P("sq", c % 2)],
                 lambda e, c=c, sq=sq: e.tensor_tensor(out=sq, in0=V[:, c, sl], in1=V[:, c, sl], op=ALU.mult))
            S.op("pe", [P("V", c), P("ONES")], [P("ps", b1)],
                 lambda e, c=c: e.matmul(C.bank(b1), lhsT=C.ONES[:], rhs=V[:, c, sl], start=(c == 0), stop=(c == NK - 1)))
            S.op("pe", [P("sq", c % 2), P("ONES")], [P("ps", b2)],
                 lambda e, c=c, sq=sq: e.matmul(C.bank(b2), lhsT=C.ONES[:], rhs=sq, start=(c == 0), stop=(c == NK - 1)))
        mean = MR[:, 0:512]
        rstd = MR[:, 512:1024]
        S.op("act", [P("ps", b1)], [P("mean")], lambda e: e.activation(out=mean, in_=C.bank(b1), func=AF.Identity, scale=1.0 / D))
        zt = ZT[0]
        S.op("dve", [P("mean")], [P("zt", 0)], lambda e: e.tensor_tensor(out=zt, in0=mean, in1=mean, op=ALU.mult))
        S.op("dve", [P("ps", b2), P("zt", 0)], [P("rstd")],
             lambda e: e.scalar_tensor_tensor(out=rstd, in0=C.bank(b2), scalar=1.0 / D, in1=zt,
                                              op0=ALU.mult, op1=ALU.subtract))
        S.op("act", [P("rstd"), P("EPST")], [P("rstd")],
             lambda e: e.activation(out=rstd, in_=rstd, func=AF.Sqrt, bias=C.EPST[:], scale=1.0))
        S.op("dve", [P("rstd")], [P("rstd")], lambda e: e.reciprocal(out=rstd, in_=rstd))
        for c in range(NK):
            z = ZT[c % 2]
            pz = P("zt", c % 2)
            S.op("dve", [P("V", c), P("mean")], [pz],
                 lambda e, c=c, z=z: e.tensor_tensor(out=z, in0=V[:, c, sl], in1=mean, op=ALU.subtract))
            S.op("dve", [pz, P("rstd")], [pz],
                 lambda e, z=z: e.tensor_tensor(out=z, in0=z, in1=rstd, op=ALU.mult))
            S.op("act", [pz, P("vec")], [P("V", c)],
                 lambda e, c=c, z=z: e.activation(out=V[:, c, sl], in_=z, func=AF.Silu,
                                                  bias=VEC[:, 2, c:c + 1], scale=VEC[:, 1, c:c + 1]))
    out_proj_ln(C, li, V, lambda c: P("V", c), dr["conf_w_out"][j], o_w, [P("win", i) for i in range(3)])


def shortconv(C, li, j):
    S, nc, dr = C.S, C.nc, C.dr
    S.barrier()
    XT = C.XT
    o = 0
    Y = C.aview(o, 8192, BF16, pat="p (c t) -> p c t", t=T); o += 8192
    WIN = [C.aview(o + i * 3072, 3072, BF16, pat="p (k n) -> p k n", n=768) for i in range(2)]; o_w = o; o += 6144
    CH = [C.aview(o + i * 2080, 2080) for i in range(2)]; o += 4160
    ACC = [C.aview(o + i * 512, 512) for i in range(2)]; o += 1024
    CS = [C.aview(o + i * 512, 512) for i in range(2)]; o += 1024
    WC = C.aview(o, 32, pat="p (c k) -> p c k", k=4); o += 32
    assert o <= ARENA_F32 - 1024
    for c in range(NK):
        S.dma("sp", [], [P("wc")], WC[:, c, 0:3], dr["sc_w_convT"][j, c * 128:(c + 1) * 128, :])
    for i in range(2):
        S.op("dve", [], [P("CH", i)], lambda e, i=i: e.memset(CH[i][:, 0:32], 0.0))
    win_d = dr["sc_w_in"][j].rearrange("(k p) n -> p k n", p=128)
    for grp in range(4):
        wt = WIN[grp % 2]
        pw = P("win", grp % 2)
        for which in range(3):
            S.dma("pool", [], [pw], wt[:, :, which * 256:(which + 1) * 256],
                  win_d[:, :, which * D + grp * 256: which * D + (grp + 1) * 256])
        for cc in range(2):
            oc = grp * 2 + cc
            chb = CH[oc % 2]
            pch = P("CH", oc % 2)
            for tb in range(4):
                i2 = (oc * 4 + tb) % 2
                banks = (0, 1, 2) if i2 == 0 else (3, 6, 7)
                for which in range(3):
                    for k in range(NK):
                        S.op("pe", [pw] + [P("XT", t4) for t4 in range(tb * 4, tb * 4 + 4)], [P("ps", banks[which])],
                             lambda e, k=k, which=which: e.matmul(
                                 C.bank(banks[which]), lhsT=wt[:, k, which * 256 + cc * 128: which * 256 + cc * 128 + 128],
                                 rhs=XT[:, k, tb * 512:(tb + 1) * 512], start=(k == 0), stop=(k == NK - 1)))
                cs = CS[i2]
                S.op("act", [P("ps", banks[1])], [P("cs", i2)],
                     lambda e: e.activation(out=cs, in_=C.bank(banks[1]), func=AF.Copy))
                S.op("dve", [P("ps", banks[2]), P("cs", i2)], [pch],
                     lambda e: e.tensor_tensor(out=chb[:, 32 + tb * 512:32 + (tb + 1) * 512], in0=C.bank(banks[2]),
                                               in1=cs, op=ALU.mult))
                acc = ACC[i2]
                base = 32 + tb * 512
                S.op("dve", [pch, P("wc")], [P("acc", i2)],
                     lambda e: e.tensor_scalar(out=acc, in0=chb[:, base - 2: base - 2 + 512], scalar1=WC[:, oc, 0:1],
                                               scalar2=None, op0=ALU.mult))
                for k in (1, 2):
                    S.op("dve", [pch, P("wc"), P("acc", i2)], [P("acc", i2)],
                         lambda e, k=k: e.scalar_tensor_tensor(out=acc, in0=chb[:, base - 2 + k: base - 2 + k + 512],
                                                               scalar=WC[:, oc, k:k + 1], in1=acc,
                                                               op0=ALU.mult, op1=ALU.add))
                S.op("dve", [P("ps", banks[0]), P("acc", i2)], [P("Y", oc)],
                     lambda e: e.tensor_tensor(out=Y[:, oc, tb * 512:(tb + 1) * 512], in0=C.bank(banks[0]), in1=acc,
                                               op=ALU.mult))
    out_proj_ln(C, li, Y, lambda c: P("Y", c), dr["sc_w_out"][j], o_w, [P("win", i) for i in range(2)])


NEG = -30000.0


NSA_STAGE = 99
NSA_OPEN = ('const', 'poolinit', 'scale', 'wn', 'w1', 'proj', 'tok')
NSA_S6 = ('dma', 'tr', 'mm')
NSA_SUB = ('bias', 'hid0', 'hid1', 'gelu', 'kcmp', 'vcmp')
ONLY_NSA = False
SKIP_LAYERS = ()


def nsa(C, li, j):
    S, dr, X, XT = C.S, C.dr, C.X, C.XT
    S.barrier()
    STG = NSA_STAGE
    o = [0]

    def alloc(n):
        off = o[0]
        o[0] += n
        return off

    av = C.aview
    o_wn = alloc(4160)
    WN = av(o_wn, 4160, BF16, pat="p (k n) -> p k n", n=1040)
    OTH = av(o_wn, 2048, BF16, pat="p (c t) -> p c t", t=T)
    WOH = av(o_wn + 2048, 1024, BF16, pat="p (c n) -> p c n", n=D)
    QT = av(alloc(2048), 2048, BF16, pat="p (c t) -> p c t", t=T)
    KSm = [av(alloc(1024), 1024, BF16) for _ in range(2)]
    KWm = [av(alloc(1024), 1024, BF16) for _ in range(2)]
    KVC = av(alloc(1024), 1024, BF16)
    VSX = av(alloc(520), 520, BF16, pat="p (t n) -> p t n", n=65)
    VWX = av(alloc(520), 520, BF16, pat="p (t n) -> p t n", n=65)
    GSIG = av(alloc(192), 192, pat="p (t n) -> p t n", n=12)
    o_o = alloc(4096)
    O = av(o_o, 4096, pat="p (t n) -> p t n", n=256)
    W1K = av(o_o, 2048, BF16, pat="p (j n) -> p j n", n=128)
    W1V = av(o_o + 2048, 2048, BF16, pat="p (j n) -> p j n", n=128)
    PET = av(alloc(17), 17, BF16)
    W2 = av(alloc(128), 128, BF16)
    BIAS = av(alloc(2), 2)
    ZT = av(alloc(256), 256)
    Z2 = av(alloc(256), 256)
    GH = av(alloc(128), 128, BF16)
    KCM = [av(alloc(64), 64, BF16) for _ in range(2)]
    VCX = av(alloc(49), 49, BF16)
    CMPMASK = av(alloc(1024), 1024, BF16)
    CAUS = av(alloc(1024), 1024, BF16, pat="p (d n) -> p d n", n=512)
    WINM = av(alloc(1024), 1024, BF16, pat="p (d n) -> p d n", n=512)
    EXP2 = av(alloc(1024), 1024, BF16, pat="p (k n) -> p k n", n=128)
    NEGSELT = av(alloc(1024), 1024, BF16)
    VNF = av(alloc(256), 256, BF16, pat="p (t n) -> p t n", n=32)
    BONUS = av(alloc(256), 256, BF16, pat="p (t n) -> p t n", n=32)
    IMP = av(alloc(512), 512, pat="p (t n) -> p t n", n=32)
    SCR = av(alloc(32), 32)
    TMPS = av(alloc(32), 32)
    M8 = av(alloc(16), 16)
    NSB = av(alloc(16), 16, BF16)
    NEB = 2
    EB = [av(alloc(256), 256, BF16) for _ in range(NEB)]
    OB = [av(alloc(128), 128, BF16) for _ in range(2)]
    DR = [av(alloc(12), 12, pat="p (a n) -> p a n", n=4) for _ in range(2)]
    assert o[0] <= ARENA_F32 - 1024, o[0]

    if 'const' in NSA_OPEN:
        S.dma("sp", [], [P("cmpmask")], CMPMASK, dr["c_cmpmask"][:, :])
        S.dma("sp", [], [P("caus")], CAUS, dr["c_caus"][:, :, :])
        S.dma("sp", [], [P("winm")], WINM, dr["c_winm"][:, :, :])
        S.dma("sp", [], [P("exp2")], EXP2[0:32], dr["c_exp2"][:, :, :])
        S.dma("sp", [], [P("vnf")], VNF, dr["c_vnf"][:, :, :])
        S.dma("sp", [], [P("bonus")], BONUS, dr["c_bonus"][:, :, :])
        S.dma("sp", [], [P("vcx")], VCX[:, 64:97], dr["c_onesovl"][:, :])
    if 'poolinit' in NSA_OPEN:
        S.op("dve", [], [P("pet")], lambda e: e.memset(PET, 0.0))
        S.dma("pool", [], [P("pet")], PET[0:64, 0:32], dr["nsa_pe_kT"][j])
        S.dma("pool", [], [P("pet")], PET[64:128, 0:32], dr["nsa_pe_vT"][j])
        S.op("dve", [], [P("w2")], lambda e: e.memset(W2[:, 64:128], 0.0))
        S.dma("pool", [], [P("w2")], W2[:, 0:64], dr["nsa_wk2"][j])
        S.dma("pool", [], [P("w2")], W2[:, 128:192], dr["nsa_wk2"][j])
        S.dma("pool", [], [P("w2")], W2[:, 192:256], dr["nsa_wv2"][j])
        S.op("dve", [], [P("zt")], lambda e: e.memset(ZT, 0.0))
        S.op("dve", [], [P("vsx")], lambda e: e.memset(VSX[:, :, 64:65], 1.0))
        S.op("dve", [], [P("vwx")], lambda e: e.memset(VWX[:, :, 64:65], 1.0))
    if 'scale' in NSA_OPEN:
        scale_x(C)
    win_d = dr["nsa_w_in"][j].rearrange("(k p) n -> p k n", p=128)
    wout_d = dr["nsa_w_out"][j]
    xt_all = [P("XT", t) for t in range(NT)]
    cnt = {"proj": 0, "A": 0, "B": 0, "E": 0, "dr": 0}

    def rot(name, n):
        v = cnt[name] % n
        cnt[name] += 1
        return v

    for hk in range(4):
        segs = [(0, hk * 256, 256), (256, 1536 + hk * 64, 64), (448, 1536 + hk * 64, 64),
                (512, 2048 + hk * 64, 64), (704, 2048 + hk * 64, 64), (768, 1024 + hk * 64, 64),
                (832, 1280 + hk * 64, 64), (896, 1792 + hk * 64, 64), (960, 2304 + hk * 64, 64),
                (1024, 2560 + hk * 12, 12)]
        if 'wn' in NSA_OPEN:
            S.op("dve", [], [P("WN")], lambda e: e.memset(WN[:, :, 320:448], 0.0))
            S.op("dve", [], [P("WN")], lambda e: e.memset(WN[:, :, 576:704], 0.0))
            for (dst, src, n) in segs:
                S.dma("pool", [], [P("WN")], WN[:, :, dst:dst + n], win_d[:, :, src:src + n])
        if 'w1' in NSA_OPEN:
            S.op("dve", [], [P("O")], lambda e: e.memset(W1K[64:128], 0.0))
            S.op("dve", [], [P("O")], lambda e: e.memset(W1V[0:64], 0.0))
            S.dma("pool", [], [P("O")], W1K[0:64], dr["nsa_wk1"][j].rearrange("(jj d) n -> d jj n", d=64))
            S.dma("pool", [], [P("O")], W1V[64:128], dr["nsa_wv1"][j].rearrange("(jj d) n -> d jj n", d=64))
        for tb in range(4 if 'proj' in NSA_OPEN else 0):
            xparts = xt_all[tb * 4: tb * 4 + 4]
            sl = slice(tb * 512, (tb + 1) * 512)
            for (c0, dstap, part, kind) in ((0, QT[:, 0, sl], P("qt"), "q"), (128, QT[:, 1, sl], P("qt"), "q"),
                                            (256, KSm[0][:, sl], P("ks"), "c"), (384, KSm[1][:, sl], P("ks"), "c"),
                                            (512, KWm[0][:, sl], P("kw"), "c"), (640, KWm[1][:, sl], P("kw"), "c"),
                                            (768, KVC[:, sl], P("kvc"), "c")):
                b = rot("proj", 2)
                for k in range(NK):
                    S.op("pe", [P("WN")] + xparts, [P("ps", b)],
                         lambda e, k=k: e.matmul(C.bank(b), lhsT=WN[:, k, c0:c0 + 128], rhs=XT[:, k, sl],
                                                 start=(k == 0), stop=(k == NK - 1)))
                if kind == "q":
                    S.op("act", [P("ps", b)], [part],
                         lambda e: e.activation(out=dstap, in_=C.bank(b), func=AF.Identity, scale=0.125))
                else:
                    S.op("dve", [P("ps", b)], [part], lambda e: e.tensor_copy(out=dstap, in_=C.bank(b)))
        for tt in range(NT if 'tok' in NSA_OPEN else 0):
            b = rot("proj", 2)
            for k in range(NK):
                S.op("pe", [P("WN"), P("XT", tt)], [P("ps", b)],
                     lambda e, k=k: e.matmul(C.bank(b)[:, 0:140], lhsT=XT[:, k, tt * 128:(tt + 1) * 128],
                                             rhs=WN[:, k, 896:1036], start=(k == 0), stop=(k == NK - 1)))
            S.op("dve", [P("ps", b)], [P("vsx")], lambda e: e.tensor_copy(out=VSX[:, tt, 0:64], in_=C.bank(b)[:, 0:64]))
            S.op("dve", [P("ps", b)], [P("vwx")], lambda e: e.tensor_copy(out=VWX[:, tt, 0:64], in_=C.bank(b)[:, 64:128]))
            S.op("act", [P("ps", b)], [P("gsig")],
                 lambda e: e.activation(out=GSIG[:, tt, :], in_=C.bank(b)[:, 128:140], func=AF.Sigmoid))
        if STG < 2:
            continue
        hb = 6
        if hk == 0 and 'bias' in NSA_SUB:
            for half in range(2):
                hs = half * 64
                for jj in range(32):
                    S.op("pe", [P("O"), P("pet")], [P("ps", 7)],
                         lambda e, jj=jj: e.matmul(C.bank(7)[:, 2 * half:2 * half + 2], lhsT=(W1K, W1V)[half][:, jj, :],
                                                   rhs=PET[:, jj:jj + 2], start=(jj == 0), stop=(jj == 31)))
            S.op("dve", [P("ps", 7)], [P("bias")], lambda e: e.tensor_copy(out=BIAS, in_=C.bank(7)[:, 0:4:2]))
        for half in range(2):
            hs = half * 64
            if ('hid%d' % half) not in NSA_SUB:
                continue
            for jj in range(32):
                S.op("pe", [P("O"), P("kvc")], [P("ps", hb)],
                     lambda e, jj=jj: e.matmul(C.bank(hb)[:, half * 128: half * 128 + 127], lhsT=(W1K, W1V)[half][:, jj, :],
                                               rhs=KVC[:, jj: jj + 2017: 16], start=(jj == 0), stop=(jj == 31)))
        if 'gelu' not in NSA_SUB:
            continue
        for half in range(2):
            zs = slice(half * 128, half * 128 + 127)
            S.op("act", [P("ps", hb), P("bias")], [P("zt")],
                 lambda e: e.activation(out=ZT[:, zs], in_=C.bank(hb)[:, zs], func=AF.Identity,
                                        bias=BIAS[:, half:half + 1], scale=1.0))
        S.op("dve", [P("zt")], [P("z2")], lambda e: e.tensor_tensor(out=Z2, in0=ZT, in1=ZT, op=ALU.mult))
        S.op("dve", [P("z2")], [P("z2")],
             lambda e: e.tensor_scalar(out=Z2, in0=Z2, scalar1=0.044715, scalar2=1.0, op0=ALU.mult, op1=ALU.add))
        S.op("dve", [P("z2"), P("zt")], [P("z2")], lambda e: e.tensor_tensor(out=Z2, in0=Z2, in1=ZT, op=ALU.mult))
        S.op("act", [P("z2")], [P("z2")],
             lambda e: e.activation(out=Z2, in_=Z2, func=AF.Sigmoid, scale=1.5957691216057308))
        S.op("dve", [P("z2"), P("zt")], [P("gh")], lambda e: e.tensor_tensor(out=GH, in0=Z2, in1=ZT, op=ALU.mult))
        if 'kcmp' not in NSA_SUB:
            continue
        S.op("pe", [P("gh"), P("w2")], [P("ps", 7)],
             lambda e: e.matmul(C.bank(7)[:, 0:127], lhsT=W2[:, 0:128], rhs=GH[:, 0:127], start=True, stop=True))
        S.op("pe", [P("gh"), P("w2")], [P("ps", 7)],
             lambda e: e.matmul(C.bank(7)[:, 128:255], lhsT=W2[:, 64:192], rhs=GH[:, 0:127], start=True, stop=True))
        S.op("pe", [P("gh"), P("w2")], [P("ps", 7)],
             lambda e: e.matmul(C.bank(7)[0:127, 256:320], lhsT=GH[:, 128:255], rhs=W2[:, 192:256], start=True, stop=True))
        S.op("dve", [P("ps", 7)], [P("kcmpt")], lambda e: e.tensor_copy(out=KCM[0][:, 0:127], in_=C.bank(7)[:, 0:127]))
        S.op("dve", [P("ps", 7)], [P("kcmpt")], lambda e: e.tensor_copy(out=KCM[1][:, 0:127], in_=C.bank(7)[:, 128:255]))
        S.op("dve", [P("ps", 7)], [P("vcx")], lambda e: e.tensor_copy(out=VCX[0:127, 0:64], in_=C.bank(7)[0:127, 256:320]))

        def post(bB, qb, g, br, ncols, first_o):
            Bv = C.bank(bB).rearrange("p (s n) -> p s n", n=128)
            d = DR[rot("dr", 2)]
            pd = P("dr", (cnt["dr"] - 1) % 2)
            S.op("dve", [P("ps", bB)], [pd],
                 lambda e: e.tensor_scalar(out=d[:, 0, :], in0=Bv[:, :, 64], scalar1=1e-30, scalar2=None, op0=ALU.max))
            S.op("dve", [pd], [pd], lambda e: e.reciprocal(out=d[:, 1, :], in_=d[:, 0, :]))
            S.op("dve", [pd, P("gsig")], [pd],
                 lambda e: e.tensor_tensor(out=d[:, 2, :], in0=d[:, 1, :], in1=GSIG[:, qb * 4:(qb + 1) * 4, g * 3 + br],
                                           op=ALU.mult))
            for s in range(4):
                tt = qb * 4 + s
                osl = O[:, tt, g * 64:(g + 1) * 64]
                if first_o:
                    S.op("dve", [P("ps", bB), pd], [P("O")],
                         lambda e: e.tensor_scalar(out=osl, in0=Bv[:, s, 0:64], scalar1=d[:, 2, s:s + 1], scalar2=None,
                                                   op0=ALU.mult))
                else:
                    S.op("dve", [P("ps", bB), pd, P("O")], [P("O")],
                         lambda e: e.scalar_tensor_tensor(out=osl, in0=Bv[:, s, 0:64], scalar=d[:, 2, s:s + 1], in1=osl,
                                                          op0=ALU.mult, op1=ALU.add))
                if ncols > 65:
                    if g == 0:
                        S.op("dve", [P("ps", bB), pd], [P("imp")],
                             lambda e: e.tensor_scalar(out=IMP[:, tt, :], in0=Bv[:, s, 65:97], scalar1=d[:, 1, s:s + 1],
                                                       scalar2=None, op0=ALU.mult))
                    else:
                        S.op("dve", [P("ps", bB), pd, P("imp")], [P("imp")],
                             lambda e: e.scalar_tensor_tensor(out=IMP[:, tt, :], in0=Bv[:, s, 65:97],
                                                              scalar=d[:, 1, s:s + 1], in1=IMP[:, tt, :],
                                                              op0=ALU.mult, op1=ALU.add))

        if STG < 3:
            continue
        for g in range(4):
            hs, qc = 64 * (g % 2), g // 2
            for qb in range(4):
                sl = slice(qb * 512, (qb + 1) * 512)
                bA = 2 + rot("A", 2)
                S.op("pe", [P("kcmpt"), P("qt")], [P("ps", bA)],
                     lambda e: e.matmul(C.bank(bA)[0:127, :], lhsT=KCM[g % 2][:, 0:127], rhs=QT[:, qc, sl],
                                        start=True, stop=False))
                S.op("pe", [P("IDB"), P("cmpmask")], [P("ps", bA)],
                     lambda e: e.matmul(C.bank(bA)[0:127, :], lhsT=C.IDB[0:127, 0:127], rhs=CMPMASK[0:127, sl],
                                        start=False, stop=True))
                ei = rot("E", NEB)
                E = EB[ei]
                S.op("act", [P("ps", bA)], [P("E", ei)],
                     lambda e: e.activation(out=E[0:127, :], in_=C.bank(bA)[0:127, :], func=AF.Exp))
                bB = 4 + rot("B", 2)
                for s in range(4):
                    S.op("pe", [P("E", ei), P("vcx")], [P("ps", bB)],
                         lambda e, s=s: e.matmul(C.bank(bB)[:, s * 128: s * 128 + 97], lhsT=E[0:127, s * 128:(s + 1) * 128],
                                                 rhs=VCX[0:127, 0:97], start=True, stop=True))
                post(bB, qb, g, 0, 97, True)
        if STG < 4:
            continue
        for tt in range(NT):
            S.op("dve", [P("imp"), P("vnf")], [P("scr")],
                 lambda e: e.tensor_tensor(out=SCR, in0=IMP[:, tt, :], in1=VNF[:, tt, :], op=ALU.mult))
            S.op("dve", [P("scr"), P("bonus")], [P("scr")],
                 lambda e: e.tensor_tensor(out=SCR, in0=SCR, in1=BONUS[:, tt, :], op=ALU.add))
            S.op("dve", [P("scr")], [P("m8")], lambda e: e.max(out=M8[:, 0:8], in_=SCR))
            S.op("dve", [P("scr"), P("m8")], [P("tmps")],
                 lambda e: e.match_replace(out=TMPS, in_to_replace=M8[:, 0:8], in_values=SCR, imm_value=-3.0e38))
            S.op("dve", [P("tmps")], [P("m8")], lambda e: e.max(out=M8[:, 8:16], in_=TMPS))
            S.op("dve", [P("scr"), P("m8")], [P("nsb")],
                 lambda e: e.tensor_scalar(out=NSB, in0=SCR, scalar1=M8[:, 15:16], scalar2=NEG, op0=ALU.is_lt, op1=ALU.mult))
            pst = C.bank(7).bitcast(BF16)
            S.op("pe", [P("nsb"), P("IDB")], [P("ps", 7)],
                 lambda e: e.transpose(out=pst[0:32, 0:128], in_=NSB, identity=C.IDB[:]))
            S.op("dve", [P("ps", 7)], [P("negselt")],
                 lambda e: e.tensor_copy(out=NEGSELT[0:32, tt * 128:(tt + 1) * 128], in_=pst[0:32, 0:128]))
        if STG < 5:
            continue
        for br in (1, 2):
            KT2, VX, pk, pv = (KSm, VSX, P("ks"), P("vsx")) if br == 1 else (KWm, VWX, P("kw"), P("vwx"))
            for g in range(4):
                hs, qc = 64 * (g % 2), g // 2
                for qb in range(4):
                    sl = slice(qb * 512, (qb + 1) * 512)
                    kt_lo = 0 if br == 1 else max(0, 4 * qb - 4)
                    bB = 4 + rot("B", 2)
                    for kt in range(kt_lo, 4 * qb + 4):
                        d = kt - 4 * qb
                        bA = 2 + rot("A", 2)
                        S.op("pe", [pk, P("qt")], [P("ps", bA)],
                             lambda e: e.matmul(C.bank(bA), lhsT=KT2[g % 2][:, kt * 128:(kt + 1) * 128],
                                                rhs=QT[:, qc, sl], start=True, stop=False))
                        if br == 1:
                            S.op("pe", [P("exp2"), P("negselt")], [P("ps", bA)],
                                 lambda e: e.matmul(C.bank(bA), lhsT=EXP2[0:32, kt, :], rhs=NEGSELT[0:32, sl],
                                                    start=False, stop=(d < 0)))
                        if d >= 0:
                            S.op("pe", [P("IDB"), P("caus")], [P("ps", bA)],
                                 lambda e: e.matmul(C.bank(bA), lhsT=C.IDB[:], rhs=CAUS[:, d, :], start=False, stop=True))
                        elif br == 2:
                            S.op("pe", [P("IDB"), P("winm")], [P("ps", bA)],
                                 lambda e: e.matmul(C.bank(bA), lhsT=C.IDB[:], rhs=WINM[:, -d - 1, :], start=False, stop=True))
                        ei = rot("E", NEB)
                        E = EB[ei]
                        S.op("act", [P("ps", bA)], [P("E", ei)],
                             lambda e: e.activation(out=E, in_=C.bank(bA), func=AF.Exp))
                        for s in range(4):
                            lo = 0 if br == 1 else max(0, 4 * qb + s - 4)
                            hi = 4 * qb + s
                            if kt < lo or kt > hi:
                                continue
                            first = (kt == kt_lo and s == 0)
                            last = (kt == 4 * qb + 3 and s == 3)
                            S.op("pe", [P("E", ei), pv], [P("ps", bB)],
                                 lambda e, s=s: e.matmul(C.bank(bB)[:, s * 128: s * 128 + 65],
                                                         lhsT=E[:, s * 128:(s + 1) * 128], rhs=VX[:, kt, :],
                                                         start=first, stop=last))
                    post(bB, qb, g, br, 65, False)
        if STG < 6:
            continue
        if 'dma' in NSA_S6:
            S.dma("pool", [], [P("WN")], WOH, wout_d[hk * 256:(hk + 1) * 256, :].rearrange("(c p) n -> p c n", p=128))
        for tt in range(NT if 'tr' in NSA_S6 else 0):
            oi = tt % 2
            S.op("dve", [P("O")], [P("ob", oi)], lambda e: e.tensor_copy(out=OB[oi], in_=O[:, tt, :]))
            tb_ = tt % 2
            pst = C.bank(tb_).bitcast(BF16)
            for c in range(2):
                S.op("pe", [P("ob", oi), P("IDB")], [P("ps", tb_)],
                     lambda e, c=c: e.transpose(out=pst[:, c * 128:(c + 1) * 128], in_=OB[oi][:, c * 128:(c + 1) * 128],
                                                identity=C.IDB[:]))
            for c in range(2):
                S.op("act", [P("ps", tb_)], [P("WN")],
                     lambda e, c=c: e.activation(out=OTH[:, c, tt * 128:(tt + 1) * 128], in_=pst[:, c * 128:(c + 1) * 128],
                                                 func=AF.Copy))
        for tt in range(NT if 'mm' in NSA_S6 else 0):
            bs = 4 if tt % 2 == 0 else 6
            for h in range(2):
                for c in range(2):
                    S.op("pe", [P("WN")], [P("ps", bs + h)],
                         lambda e, c=c, h=h: e.matmul(C.bank(bs + h), lhsT=OTH[:, c, tt * 128:(tt + 1) * 128],
                                                      rhs=WOH[:, c, h * 512:(h + 1) * 512], start=(c == 0), stop=(c == 1)))
            S.op("dve", [P("ps", bs), P("ps", bs + 1), P("X", tt)], [P("X", tt)],
                 lambda e: e.tensor_tensor(out=X[:, tt, :], in0=C.PS[:, bs * 512: bs * 512 + 1024], in1=X[:, tt, :],
                                           op=ALU.add))
    load_ln(C, dr["ln_mix_g"][li], dr["ln_mix_b"][li])
    for tt in range(NT):
        ln_tail(C, tt)


def ffn_core(C, groups, gate_col):
    S, X, XT = C.S, C.X, C.XT
    NB = 3
    o = 0
    WG = [C.aview(o + i * 2048, 2048, BF16, pat="p (k n) -> p k n", n=512) for i in range(NB)]; o += NB * 2048
    WU = [C.aview(o + i * 2048, 2048, BF16, pat="p (k n) -> p k n", n=512) for i in range(NB)]; o += NB * 2048
    WD = [C.aview(o + i * 2048, 2048, BF16, pat="p (c n) -> p c n", n=D) for i in range(NB)]; o += NB * 2048
    HT = [C.aview(o + i * 1024, 1024, BF16, pat="p (c n) -> p c n", n=512) for i in range(2)]; o += 2048
    SGT = [C.aview(o + i * 512, 512) for i in range(2)]; o += 1024
    assert o <= ARENA_F32 - 1024
    it = 0
    for gi, (wg_d, wu_d, wd_d, c0, G, e) in enumerate(groups):
        bi = gi % NB
        wg, wu, wd = WG[bi], WU[bi], WD[bi]
        pwg, pwu, pwd = P("wg", bi), P("wu", bi), P("wd", bi)
        S.dma("pool", [], [pwg], wg[:, :, 0:G * 128],
              wg_d[:, c0 * 128:(c0 + G) * 128].rearrange("(k p) n -> p k n", p=128))
        S.dma("pool", [], [pwu], wu[:, :, 0:G * 128],
              wu_d[:, c0 * 128:(c0 + G) * 128].rearrange("(k p) n -> p k n", p=128))
        S.dma("pool", [], [pwd], wd[:, 0:G, :],
              wd_d[c0 * 128:(c0 + G) * 128, :].rearrange("(c p) n -> p c n", p=128))
        for tb in range(4):
            ht = HT[tb % 2]
            pht = lambda c: P("ht", tb % 2, c)
            xparts = [P("XT", t4) for t4 in range(tb * 4, tb * 4 + 4)]
            for c in range(G):
                bset = 2 * (it % 2)
                it += 1
                for which, (w, pw) in enumerate(((wg, pwg), (wu, pwu))):
                    for k in range(NK):
                        S.op("pe", [pw] + xparts, [P("ps", bset + which)],
                             lambda e, k=k, w=w, which=which: e.matmul(
                                 C.bank(bset + which), lhsT=w[:, k, c * 128:(c + 1) * 128],
                                 rhs=XT[:, k, tb * 512:(tb + 1) * 512], start=(k == 0), stop=(k == NK - 1)))
                sg = SGT[it % 2]
                psg = P("sgt", it % 2)
                S.op("act", [P("ps", bset)], [psg], lambda e: e.activation(out=sg, in_=C.bank(bset), func=AF.Silu))
                S.op("dve", [P("ps", bset + 1), psg], [pht(c)],
                     lambda e: e.tensor_tensor(out=ht[:, c, :], in0=C.bank(bset + 1), in1=sg, op=ALU.mult))
            for sub in range(4):
                tt = tb * 4 + sub
                bs = 4 + 2 * (tt % 2)
                for h in range(2):
                    for c in range(G):
                        S.op("pe", [pht(c), pwd], [P("ps", bs + h)],
                             lambda e, c=c, h=h: e.matmul(C.bank(bs + h), lhsT=ht[:, c, sub * 128:(sub + 1) * 128],
                                                          rhs=wd[:, c, h * 512:(h + 1) * 512],
                                                          start=(c == 0), stop=(c == G - 1)))
                sc = 1.0 if gate_col is None else gate_col(tt, e)
                rd = [P("ps", bs), P("ps", bs + 1), P("X", tt)] + ([] if gate_col is None else [P("gates")])
                S.op("dve", rd, [P("X", tt)],
                     lambda e: e.scalar_tensor_tensor(out=X[:, tt, :], in0=C.PS[:, bs * 512:bs * 512 + 1024], scalar=sc,
                                                      in1=X[:, tt, :], op0=ALU.mult, op1=ALU.add))


def scale_x(C):
    S, X = C.S, C.X
    for tt in range(NT):
        S.op("act", [P("X", tt)], [P("X", tt)],
             lambda e, tt=tt: e.activation(out=X[:, tt, :], in_=X[:, tt, :], func=AF.Identity, scale=ALPHA))


def mk_groups(wg, wu, wd, F, e):
    nch = F // 128
    groups = []
    c0 = 0
    while c0 < nch:
        G = min(4, nch - c0)
        groups.append((wg, wu, wd, c0, G, e))
        c0 += G
    return groups


def ffn(C, li, ws, _):
    S, dr = C.S, C.dr
    S.barrier()
    wg, wu, wd, F = ws[0]
    scale_x(C)
    ffn_core(C, mk_groups(wg, wu, wd, F, 0), None)
    load_ln(C, dr["ln_ffn_g"][li], dr["ln_ffn_b"][li])
    for tt in range(NT):
        ln_tail(C, tt)


def moe(C, li, f):
    S, dr, X, XT = C.S, C.dr, C.X, C.XT
    S.barrier()
    o = ARENA_F32 - 1024 - 1024
    WR = C.aview(o, 32, BF16, pat="p (k n) -> p k n", n=8); o += 32
    GATES = C.aview(o, 128, pat="p (t n) -> p t n", n=8); o += 128
    LG = C.aview(o, 128, pat="p (t n) -> p t n", n=8); o += 128
    M8 = C.aview(o, 128, pat="p (t n) -> p t n", n=8); o += 128
    TMP = C.aview(o, 64, pat="p (t n) -> p t n", n=4); o += 64
    S.dma("pool", [], [P("wr")], WR[:], dr["moe_w_router"][f].rearrange("(k p) n -> p k n", p=128))
    for tt in range(NT):
        b = tt % 2
        for k in range(NK):
            S.op("pe", [P("wr"), P("XT", tt)], [P("ps", b)],
                 lambda e, k=k: e.matmul(C.bank(b)[:, 0:8], lhsT=XT[:, k, tt * 128:(tt + 1) * 128], rhs=WR[:, k, :],
                                         start=(k == 0), stop=(k == NK - 1)))
        lg = LG[:, tt, :]
        m8 = M8[:, tt, :]
        pg = P("gsm", tt)
        S.op("dve", [P("ps", b)], [pg], lambda e: e.tensor_copy(out=lg, in_=C.bank(b)[:, 0:8]))
        S.op("dve", [pg], [pg], lambda e: e.max(out=m8, in_=lg))
        negm1 = TMP[:, tt, 0:1]
        e21 = TMP[:, tt, 1:2]
        r = TMP[:, tt, 2:3]
        S.op("dve", [pg], [pg], lambda e: e.tensor_scalar(out=negm1, in0=m8[:, 0:1], scalar1=-1.0, scalar2=None, op0=ALU.mult))
        S.op("act", [pg], [pg], lambda e: e.activation(out=e21, in_=m8[:, 1:2], func=AF.Exp, bias=negm1, scale=1.0))
        S.op("dve", [pg], [pg], lambda e: e.tensor_scalar(out=e21, in0=e21, scalar1=1.0, scalar2=None, op0=ALU.add))
        S.op("dve", [pg], [pg], lambda e: e.reciprocal(out=r, in_=e21))
        gt = GATES[:, tt, :]
        S.op("act", [pg], [P("gates")], lambda e: e.activation(out=gt, in_=lg, func=AF.Exp, bias=negm1, scale=1.0))
        S.op("dve", [pg], [pg], lambda e: e.tensor_scalar(out=lg, in0=lg, scalar1=m8[:, 1:2], scalar2=None, op0=ALU.is_ge))
        S.op("dve", [pg, P("gates")], [P("gates")],
             lambda e: e.scalar_tensor_tensor(out=gt, in0=gt, scalar=r, in1=lg, op0=ALU.mult, op1=ALU.mult))
    scale_x(C)
    groups = []
    for e in range(NE):
        groups += mk_groups(dr["moe_w_gate"][f, e], dr["moe_w_up"][f, e], dr["moe_w_down"][f, e], D_FFE, e)
    ffn_core(C, groups, lambda tt, e: GATES[:, tt, e:e + 1])
    load_ln(C, dr["ln_ffn_g"][li], dr["ln_ffn_b"][li])
    for tt in range(NT):
        ln_tail(C, tt)


def prep_inputs(inputs):
    f = lambda a: np.ascontiguousarray(np.asarray(a, dtype=np.float32))
    sh = {}
    for n in ("ln_mix_g", "ln_mix_b", "ln_ffn_g", "ln_ffn_b", "conf_w_in", "conf_w_out", "sc_w_in", "sc_w_out",
              "ffn_w_gate", "ffn_w_up", "ffn_w_down", "moe_w_router", "moe_w_gate", "moe_w_up", "moe_w_down"):
        sh[n] = f(inputs[n])
    sh["conf_w_dwT"] = f(np.transpose(np.asarray(inputs["conf_w_dw"]), (0, 2, 1)))
    sh["sc_w_convT"] = f(np.transpose(np.asarray(inputs["sc_w_conv"]), (0, 2, 1)))
    vec = np.stack([np.asarray(inputs["conf_b_dw"]), np.asarray(inputs["conf_ln_g"]), np.asarray(inputs["conf_ln_b"])], 1)
    sh["conf_vec"] = f(vec.reshape(2, 3, NK, 128).transpose(0, 3, 1, 2))
    for n in ("nsa_w_in", "nsa_wk1", "nsa_wv1", "nsa_wk2", "nsa_wv2", "nsa_w_out"):
        sh[n] = f(inputs[n])
    sh["nsa_pe_kT"] = f(np.transpose(np.asarray(inputs["nsa_pe_k"]), (0, 2, 1)))
    sh["nsa_pe_vT"] = f(np.transpose(np.asarray(inputs["nsa_pe_v"]), (0, 2, 1)))
    bf = lambda a: np.ascontiguousarray(a.astype(np.float32)).astype(ml_dtypes.bfloat16)
    NEGV = -30000.0
    p = np.arange(128)
    cm = np.where((16 * p[:, None] + 31) <= np.arange(2048)[None, :], 0.0, NEGV)
    sh["c_cmpmask"] = bf(cm)
    jj = np.arange(512)
    caus = np.zeros((128, 4, 512), np.float32)
    winm = np.zeros((128, 4, 512), np.float32)
    for d in range(4):
        caus[:, d, :] = np.where(128 * d + p[:, None] <= jj[None, :], 0.0, NEGV)
        winm[:, d, :] = np.where(jj[None, :] + 128 * (d + 1) - p[:, None] < 512, 0.0, NEGV)
    sh["c_caus"] = bf(caus)
    sh["c_winm"] = bf(winm)
    e2 = np.zeros((32, 16, 128), np.float32)
    for kt in range(16):
        for pp in range(128):
            e2[2 * kt + pp // 64, kt, pp] = 1.0
    sh["c_exp2"] = bf(e2)
    tpos = (np.arange(16)[None, :] * 128 + p[:, None])
    nb = np.arange(32)
    dcur = tpos[:, :, None] // 64 - nb[None, None, :]
    forced = (nb[None, None, :] == 0) | ((dcur >= 0) & (dcur < 2))
    valid = (nb[None, None, :] * 64) <= tpos[:, :, None]
    sh["c_vnf"] = bf((valid & ~forced).astype(np.float32))
    sh["c_bonus"] = bf(np.where(forced, 1e4, np.where(valid, 0.0, -1e30)).astype(np.float32))
    cs = np.arange(128) * 16
    ovl = ((cs[:, None] < (nb * 64 + 64)[None, :]) & ((cs + 32)[:, None] > (nb * 64)[None, :])).astype(np.float32)
    ovl[127, :] = 0.0
    sh["c_onesovl"] = bf(np.concatenate([np.ones((128, 1), np.float32), ovl], 1))
    sh["ident_bf"] = np.eye(128, dtype=np.float32).astype(ml_dtypes.bfloat16)
    sh["ones_bf"] = np.ones((128, 128), dtype=np.float32).astype(ml_dtypes.bfloat16)
    return sh


def run(inputs, n_layers=DEPTH, cores=8, trace=False, layers=None, x=None, sh=None):
    nc, C = build_program(n_layers, layers)
    if sh is None:
        sh = prep_inputs(inputs)
    if x is None:
        x = np.asarray(inputs["x"], dtype=np.float32)
    in_maps = []
    for c in range(cores):
        m = dict(sh)
        m["x"] = np.ascontiguousarray(x[c])
        in_maps.append(m)
    res = run_bass_kernel_spmd(nc, in_maps, core_ids=list(range(cores)), trace=trace)
    outs = np.stack([np.asarray(r["out"], dtype=np.float32) for r in res.results], 0)
    return outs, res


LAUNCH_GROUPS = [[0, 1], [2], [3]]


def kernel(**inputs):
    sh = prep_inputs(inputs)
    x = np.asarray(inputs["x"], dtype=np.float32)
    for grp in LAUNCH_GROUPS:
        x, _ = run(inputs, DEPTH, 8, layers=grp, x=x, sh=sh)
    return x
```
